# Optimizing a Trainium2 kernel written in Bass

```python
import math
import jax, jax.numpy as jnp
from jax import lax
import numpy as np

D_MODEL = 2048
BATCH = 4
SEQ = 2048
DEPTH = 2

N_A = DEPTH // 2
N_B = DEPTH - N_A
GROUP_CH = 16
N_GROUPS = D_MODEL // GROUP_CH
STATE = 64
DT_MIN = 1e-3
DT_MAX = 1e-1
HEAD_DIM = 128
N_HEADS = D_MODEL // HEAD_DIM
BLOCK = 256
TOP_K_BLOCKS = 3
Q_CHUNK = 32
D_FF = int(math.ceil(8 * D_MODEL / 3 / 256) * 256)
EPS = 1e-6

kernel_name = "yoco_s5_moba_hybrid"


def rmsnorm(x, g):
    xf = x.astype(jnp.float32)
    y = xf * lax.rsqrt(jnp.mean(xf * xf, axis=-1, keepdims=True) + EPS)
    return (y * g.astype(jnp.float32)).astype(x.dtype)


def swiglu(u, w_gate, w_up, w_down):
    return (jax.nn.silu(u @ w_gate) * (u @ w_up)) @ w_down


def _ssm_combine(e1, e2):
    a1, b1 = e1
    a2, b2 = e2
    return a2 * a1, a2 * b1 + b2


def s5_mixer(u, lam_re, lam_im, log_dt, b_re, b_im, c_re, c_im, d_skip, w_glu, b_glu):
    bsz, L, D = u.shape
    f32 = jnp.float32
    lam = lax.complex(lam_re.astype(f32), lam_im.astype(f32))
    dt = jnp.exp(log_dt.astype(f32))[:, None]
    lam_bar = jnp.exp(lam * dt)
    b_c = lax.complex(b_re.astype(f32), b_im.astype(f32))
    b_bar = ((lam_bar - 1.0) / lam)[..., None] * b_c
    c_c = lax.complex(c_re.astype(f32), c_im.astype(f32))
    uf = u.astype(f32)
    ug = uf.reshape(bsz, L, N_GROUPS, GROUP_CH).astype(jnp.complex64)
    bu = jnp.einsum('blgc,gpc->blgp', ug, b_bar)
    a = jnp.broadcast_to(lam_bar, (1, L) + lam_bar.shape)
    _, states = lax.associative_scan(_ssm_combine, (a, bu), axis=1)
    y = jnp.einsum('blgp,gcp->blgc', states, c_c).real.reshape(bsz, L, D)
    y = y + d_skip.astype(f32) * uf
    z = jax.nn.gelu(y).astype(u.dtype)
    return z * jax.nn.sigmoid(z @ w_glu + b_glu)


def shared_kv(h, g_kv, w_kv):
    bsz, L, D = h.shape
    nb = -(-L // BLOCK)
    pad = nb * BLOCK - L
    kv = rmsnorm(h, g_kv) @ w_kv
    k, v = jnp.split(kv, 2, axis=-1)

    def to_blocks(t):
        t = jnp.pad(t, ((0, 0), (0, pad), (0, 0)))
        return t.reshape(bsz, nb, BLOCK, N_HEADS, HEAD_DIM).transpose(0, 3, 1, 2, 4)

    k_blocks = to_blocks(k)
    v_blocks = to_blocks(v)
    k_mean = jnp.mean(k_blocks.astype(jnp.float32), axis=3).astype(h.dtype)
    return k_blocks, v_blocks, k_mean


def alibi_slopes(n):
    return jnp.exp2(-8.0 * jnp.arange(1, n + 1, dtype=jnp.float32) / n)


def moba_attention(q, k_blocks, v_blocks, k_mean):
    bsz, H, L, Dh = q.shape
    nb = k_blocks.shape[2]
    k_sel = min(TOP_K_BLOCKS, nb)
    scale = Dh ** -0.5
    slopes = alibi_slopes(H)
    b_ix = jnp.arange(bsz)[:, None, None, None]
    h_ix = jnp.arange(H)[None, :, None, None]
    blk_ar = jnp.arange(BLOCK)

    def chunk(c):
        t0 = c * Q_CHUNK
        qc = lax.dynamic_slice_in_dim(q, t0, Q_CHUNK, axis=2)
        t = t0 + jnp.arange(Q_CHUNK)
        own = t0 // BLOCK
        gate = jnp.einsum('bhtd,bhnd->bhtn', qc, k_mean).astype(jnp.float32)
        gate = jnp.where(jnp.arange(nb) < own, gate, -jnp.inf)
        _, idx = lax.top_k(gate, k_sel)
        valid = jnp.arange(k_sel) < own
        k_g = k_blocks[b_ix, h_ix, idx]
        v_g = v_blocks[b_ix, h_ix, idx]
        s_sel = jnp.einsum('bhtd,bhtjsd->bhtjs', qc, k_g).astype(jnp.float32) * scale
        pos_sel = idx[..., None] * BLOCK + blk_ar
        dist_sel = (t[:, None, None] - pos_sel).astype(jnp.float32)
        s_sel = s_sel - slopes.reshape(1, H, 1, 1, 1) * dist_sel
        s_sel = jnp.where(valid[:, None], s_sel, -jnp.inf)
        k_own = lax.dynamic_slice_in_dim(k_blocks, own, 1, axis=2)[:, :, 0]
        v_own = lax.dynamic_slice_in_dim(v_blocks, own, 1, axis=2)[:, :, 0]
        s_own = jnp.einsum('bhtd,bhsd->bhts', qc, k_own).astype(jnp.float32) * scale
        dist_own = t[:, None] - (own * BLOCK + blk_ar)[None, :]
        s_own = jnp.where(dist_own >= 0,
                          s_own - slopes.reshape(1, H, 1, 1) * dist_own.astype(jnp.float32),
                          -jnp.inf)
        s_all = jnp.concatenate([s_sel.reshape(bsz, H, Q_CHUNK, k_sel * BLOCK), s_own], axis=-1)
        p = jax.nn.softmax(s_all, axis=-1).astype(q.dtype)
        p_sel = p[..., :k_sel * BLOCK].reshape(bsz, H, Q_CHUNK, k_sel, BLOCK)
        p_own = p[..., k_sel * BLOCK:]
        return (jnp.einsum('bhtjs,bhtjsd->bhtd', p_sel, v_g)
                + jnp.einsum('bhts,bhsd->bhtd', p_own, v_own))

    out = lax.map(chunk, jnp.arange(L // Q_CHUNK))
    return out.transpose(1, 0, 3, 2, 4).reshape(bsz, L, H * Dh)


def setup_inputs(seed: int = 0) -> dict:
    key = jax.random.key(seed)
    ks = jax.random.split(key, 24)
    D, F, G, P, C = D_MODEL, D_FF, N_GROUPS, STATE, GROUP_CH
    f32 = jnp.float32

    def nrm(k, shape, scale):
        return jax.random.normal(k, shape, f32) * scale

    x = jax.random.normal(ks[0], (BATCH, SEQ, D), f32)
    g_mix = 1.0 + nrm(ks[1], (DEPTH, D), 0.02)
    lambda_re = -0.5 + nrm(ks[2], (N_A, G, P), 0.01)
    lambda_im = jnp.pi * jnp.broadcast_to(jnp.arange(P, dtype=f32), (N_A, G, P)) + nrm(ks[3], (N_A, G, P), 0.01)
    log_dt = jax.random.uniform(ks[4], (N_A, G), f32, math.log(DT_MIN), math.log(DT_MAX))
    b_re = nrm(ks[5], (N_A, G, P, C), (2 * C) ** -0.5)
    b_im = nrm(ks[6], (N_A, G, P, C), (2 * C) ** -0.5)
    c_re = nrm(ks[7], (N_A, G, C, P), P ** -0.5)
    c_im = nrm(ks[8], (N_A, G, C, P), P ** -0.5)
    d_skip = nrm(ks[9], (N_A, D), 1.0)
    w_glu = nrm(ks[10], (N_A, D, D), D ** -0.5)
    b_glu = nrm(ks[11], (N_A, D), 0.02)
    g_kv = 1.0 + nrm(ks[12], (D,), 0.02)
    w_kv = nrm(ks[13], (D, 2 * D), D ** -0.5)
    w_q = nrm(ks[14], (N_B, D, D), D ** -0.5)
    w_o = nrm(ks[15], (N_B, D, D), D ** -0.5)
    g_ffn = 1.0 + nrm(ks[16], (DEPTH, D), 0.02)
    w_gate = nrm(ks[17], (DEPTH, D, F), D ** -0.5)
    w_up = nrm(ks[18], (DEPTH, D, F), D ** -0.5)
    w_down = nrm(ks[19], (DEPTH, F, D), F ** -0.5)
    g_final = 1.0 + nrm(ks[20], (D,), 0.02)
    return {"x": x, "g_mix": g_mix, "lambda_re": lambda_re, "lambda_im": lambda_im,
            "log_dt": log_dt, "b_re": b_re, "b_im": b_im, "c_re": c_re, "c_im": c_im,
            "d_skip": d_skip, "w_glu": w_glu, "b_glu": b_glu, "g_kv": g_kv, "w_kv": w_kv,
            "w_q": w_q, "w_o": w_o, "g_ffn": g_ffn, "w_gate": w_gate, "w_up": w_up,
            "w_down": w_down, "g_final": g_final}


def reference(x, g_mix, lambda_re, lambda_im, log_dt, b_re, b_im, c_re, c_im, d_skip, w_glu, b_glu,
              g_kv, w_kv, w_q, w_o, g_ffn, w_gate, w_up, w_down, g_final):
    bsz, L, D = x.shape
    h = x
    k_blocks = v_blocks = k_mean = None
    for i in range(DEPTH):
        u = rmsnorm(h, g_mix[i])
        if i < N_A:
            h = h + s5_mixer(u, lambda_re[i], lambda_im[i], log_dt[i], b_re[i], b_im[i],
                             c_re[i], c_im[i], d_skip[i], w_glu[i], b_glu[i])
        else:
            if i == N_A:
                k_blocks, v_blocks, k_mean = shared_kv(h, g_kv, w_kv)
            j = i - N_A
            q = (u @ w_q[j]).reshape(bsz, L, N_HEADS, HEAD_DIM).transpose(0, 2, 1, 3)
            h = h + moba_attention(q, k_blocks, v_blocks, k_mean) @ w_o[j]
        u = rmsnorm(h, g_ffn[i])
        h = h + swiglu(u, w_gate[i], w_up[i], w_down[i])
    return rmsnorm(h, g_final)
```

```python
from contextlib import ExitStack
import numpy as np
import concourse.bass as bass
import concourse.mybir as mybir
from concourse.bass_utils import run_bass_kernel_spmd

F32 = mybir.dt.float32
F32R = mybir.dt.float32r
I32 = mybir.dt.int32
AF = mybir.ActivationFunctionType
ALU = mybir.AluOpType

D = 2048
NT = 1024
DC = 16
FF = 5632
NFC = 44
EPS = 1e-6


class Res:
    __slots__ = ("name", "w", "reads")

    def __init__(s, name=""):
        s.name = name
        s.w = None
        s.reads = []


class Sched:
    def __init__(s, nc):
        s.nc = nc
        s.engs = {'pe': nc.tensor, 'act': nc.scalar, 'dve': nc.vector, 'pool': nc.gpsimd, 'sp': nc.sync}
        s.sem = {k: nc.alloc_semaphore(name="s_" + k) for k in s.engs}
        s.cnt = {k: 0 for k in s.engs}
        s.seen = {k: {} for k in s.engs}
        s.dma_sems = {}

    def _wait(s, eng, ev):
        if ev is None:
            return
        key, val = ev
        if s.seen[eng].get(key, 0) >= val:
            return
        s.engs[eng].wait_ge(s.sem[key], val)
        s.seen[eng][key] = val

    def op(s, eng, fn, reads=(), writes=(), inc=True):
        for r in reads:
            s._wait(eng, r.w)
        for w in writes:
            if w.w is not None and w.w[0] != eng:
                s._wait(eng, w.w)
            for ev in w.reads:
                if ev[0] != eng:
                    s._wait(eng, ev)
        ins = fn()
        if inc:
            ins.then_inc(s.sem[eng], 1)
            s.cnt[eng] += 1
            ev = (eng, s.cnt[eng])
        else:
            ev = (eng, s.cnt[eng] + 1)
        for w in writes:
            w.w = ev
            w.reads = []
        for r in reads:
            if r not in writes:
                r.reads.append(ev)
                if len(r.reads) > 12:
                    r.reads = s._compact(r.reads)
        return ins

    @staticmethod
    def _compact(evs):
        best = {}
        for k, v in evs:
            if best.get(k, 0) < v:
                best[k] = v
        return list(best.items())

    def dma(s, eng, slot, out, in_, reads=(), writes=()):
        for r in reads:
            s._wait(eng, r.w)
        for w in writes:
            s._wait(eng, w.w)
            for ev in w.reads:
                s._wait(eng, ev)
        if slot not in s.dma_sems:
            s.sem[slot] = s.nc.alloc_semaphore(name=slot)
            s.dma_sems[slot] = 0
        ins = s.engs[eng].dma_start(out=out, in_=in_)
        s.dma_sems[slot] += 16
        ins.then_inc(s.sem[slot], 16)
        ev = (slot, s.dma_sems[slot])
        for w in writes:
            w.w = ev
            w.reads = []
        for r in reads:
            r.reads.append(ev)
        return ev


class T:
    def __init__(s, nc, name, shape, dtype=F32, psum=False):
        s.t = (nc.alloc_psum_tensor if psum else nc.alloc_sbuf_tensor)(name, list(shape), dtype)
        s.r = Res(name)
        s.shape = shape

    def ap(s):
        return s.t.ap()


TWO_PI = 6.2831853
NSLOT = 24


def barrier(S):
    for e in ('pe', 'act', 'dve', 'pool', 'sp'):
        for k in ('pe', 'act', 'dve', 'pool', 'sp'):
            if k != e and S.cnt[k] > 0:
                S._wait(e, (k, S.cnt[k]))
        for slot, v in S.dma_sems.items():
            if v > 0:
                S._wait(e, (slot, v))


def bc(ap, axis, shape):
    return ap.unsqueeze(axis).to_broadcast(list(shape))


def rmsnorm_fm(S, nc, C, h, gcol_ap, u, hres=None):
    hres = hres or [h.r]
    for dc in range(16):
        sq = C.sq[dc % 2]
        S.op('act', lambda: nc.scalar.activation(out=sq.ap(), in_=h.ap()[:, dc, :], func=AF.Square),
             reads=hres, writes=[sq.r])
        for th in range(2):
            S.op('pe', lambda: nc.tensor.matmul(C.ps[th].ap(), C.ones.ap(), sq.ap()[:, th * 512:(th + 1) * 512],
                                                start=(dc == 0), stop=(dc == 15)),
                 reads=[sq.r, C.ones.r], writes=[C.ps[th].r])
    for th in range(2):
        sl = slice(th * 512, (th + 1) * 512)
        S.op('act', lambda: nc.scalar.activation(out=C.rbc.ap()[:, sl], in_=C.ps[th].ap(), func=AF.Sqrt,
                                                 bias=C.epsc.ap(), scale=1.0 / D),
             reads=[C.ps[th].r, C.epsc.r], writes=[C.rbc.r])
    S.op('dve', lambda: nc.vector.reciprocal(out=C.rbc.ap(), in_=C.rbc.ap()), reads=[C.rbc.r], writes=[C.rbc.r])
    for dc in range(16):
        S.op('dve', lambda: nc.vector.scalar_tensor_tensor(out=u.ap()[:, dc, :], in0=h.ap()[:, dc, :],
                                                           scalar=gcol_ap[:, dc:dc + 1], in1=C.rbc.ap(),
                                                           op0=ALU.mult, op1=ALU.mult),
             reads=hres + [C.rbc.r, C.gc.r], writes=[u.r])


def ffn(S, nc, C, h, u, wgu_d, wd_d):
    hs = [[Res() for _ in range(2)] for _ in range(16)]

    def load(fc):
        sl = fc % 2
        S.dma('pool', 'wgu%d' % sl, C.wgu[sl].ap(), wgu_d[fc], writes=[C.wgu[sl].r])
        S.dma('pool', 'wd%d' % sl, C.wd[sl].ap(), wd_d[fc], writes=[C.wd[sl].r])

    load(0)
    for fc in range(NFC):
        sl = fc % 2
        wgu, wd, act = C.wgu[sl], C.wd[sl], C.act[sl]
        if fc + 1 < NFC:
            load(fc + 1)
        for th in range(2):
            tsl = slice(th * 512, (th + 1) * 512)
            for gu in range(2):
                ps = C.ps[th * 2 + gu]
                for dc in range(16):
                    S.op('pe', lambda: nc.tensor.matmul(ps.ap(), wgu.ap()[:, gu, dc, :], u.ap()[:, dc, tsl],
                                                        start=(dc == 0), stop=(dc == 15)),
                         reads=[wgu.r, u.r], writes=[ps.r], inc=(dc == 15))
            sil = C.sil[th]
            S.op('act', lambda: nc.scalar.activation(out=sil.ap(), in_=C.ps[th * 2].ap(), func=AF.Silu),
                 reads=[C.ps[th * 2].r], writes=[sil.r])
            S.op('dve', lambda: nc.vector.tensor_tensor(out=act.ap()[:, tsl], in0=sil.ap(), in1=C.ps[th * 2 + 1].ap(),
                                                        op=ALU.mult),
                 reads=[sil.r, C.ps[th * 2 + 1].r], writes=[act.r])
        for dc in range(16):
            for th in range(2):
                tsl = slice(th * 512, (th + 1) * 512)
                ps = C.ps[4 + (dc * 2 + th) % 4]
                S.op('pe', lambda: nc.tensor.matmul(ps.ap(), wd.ap()[:, dc * 128:(dc + 1) * 128], act.ap()[:, tsl],
                                                    start=True, stop=True),
                     reads=[wd.r, act.r], writes=[ps.r])
                S.op('dve', lambda: nc.vector.tensor_tensor(out=h.ap()[:, dc, tsl], in0=h.ap()[:, dc, tsl],
                                                            in1=ps.ap(), op=ALU.add),
                     reads=[ps.r, hs[dc][th]], writes=[hs[dc][th]])
    return hs


STOP = 99


def s5_mixer(S, nc, C, D_, Z, es):
    def mk(name, shape, dt=F32):
        t = T.__new__(T)
        t.t = es.enter_context(nc.sbuf_tensor("sb_" + name, list(shape), dt))
        t.r = Res(name)
        t.shape = shape
        return t
    NB = 4
    NPB = 64 // NB
    Vv = mk("Vv", [128, 2, 64, 128])
    lamre, lamim, logdt = mk("lamre", [128, 64]), mk("lamim", [128, 64]), mk("logdt", [128, 64])
    Bre, Bim = mk("Bre", [128, 64, 16]), mk("Bim", [128, 64, 16])
    Cre, Cim = mk("Cre", [128, 64, 16]), mk("Cim", [128, 64, 16])
    gmixc, dskipc = mk("gmixc", [128, 128]), mk("dskipc", [128, 128])
    svec, maskA = mk("svec", [128, NSLOT]), mk("maskA", [128, 128])
    ind = mk("ind", [128, 128], F32R)
    for t, nm in ((lamre, "lamre"), (lamim, "lamim"), (logdt, "logdt"), (Bre, "Bre"), (Bim, "Bim"), (Cre, "Cre"),
                  (Cim, "Cim"), (gmixc, "gmixc"), (dskipc, "dskipc"), (svec, "svec"), (maskA, "maskA")):
        S.dma('sp', 'ld_' + nm, t.ap(), D_[nm], writes=[t.r])
    S.dma('pool', 'ld_ind', ind.ap(), D_["ind"], writes=[ind.r])
    xc_d = D_["xc"]
    V = lambda *a, **k: S.op('dve', *a, **k)
    AC = lambda *a, **k: S.op('act', *a, **k)
    tt = nc.vector.tensor_tensor
    stt = nc.vector.scalar_tensor_tensor

    xcb = [mk("xcb%d" % i, [128, 4, 256]) for i in range(2)]
    Rt = mk("Rt", [128, 256])
    shX = [128, NB, 16, 16]
    Xre, Xim = mk("Xre", shX, F32R), mk("Xim", shX, F32R)
    xsq = [Xre, Xim]
    xsq_ap = [x.ap().rearrange("p a b c -> p (a b c)").rearrange("p (g n) -> p g n", g=4) for x in xsq]
    for gb in range(32):
        b = gb % 2
        S.dma('sp', 'xcb%d' % b, xcb[b].ap(), xc_d[:, gb * 4:(gb + 1) * 4, :], writes=[xcb[b].r])
        AC(lambda: nc.scalar.activation(out=xsq_ap[b], in_=xcb[b].ap(), func=AF.Square),
           reads=[xcb[b].r], writes=[xsq[b].r])
        for g in range(4):
            S.op('pe', lambda: nc.tensor.matmul(C.ps[0].ap()[:, 0:256], ind.ap(), xsq_ap[b][:, g, :],
                                                start=(gb == 0 and g == 0), stop=(gb == 31 and g == 3)),
                 reads=[ind.r, xsq[b].r], writes=[C.ps[0].r])
    AC(lambda: nc.scalar.activation(out=Rt.ap(), in_=C.ps[0].ap()[:, 0:256], func=AF.Sqrt,
                                    bias=C.epsc.ap(), scale=1.0 / D),
       reads=[C.ps[0].r, C.epsc.r], writes=[Rt.r])
    V(lambda: nc.vector.reciprocal(out=Rt.ap(), in_=Rt.ap()), reads=[Rt.r], writes=[Rt.r])
    barrier(S)
    zTap = Z.ap().rearrange("p a b c -> p (a b c)").rearrange("p (d t) -> p d t", d=16)
    if STOP <= 1:
        return zTap

    sh3 = [128, 64, NSLOT]
    ERE, EIM = mk("ERE", sh3), mk("EIM", sh3)
    GRE, GIM = mk("GRE", [128, 64, 8]), mk("GIM", [128, 64, 8])
    vflat = Vv.ap().rearrange("p a b c -> p (a b c)")
    class TV:
        def __init__(s_, off, shape, dt=F32):
            n = int(np.prod(shape[1:]))
            a = vflat[:, off:off + n]
            if dt != F32:
                a = a.bitcast(dt)
            if len(shape) == 3:
                a = a.rearrange("p (a b) -> p a b", a=shape[1])
            s_._ap = a
            s_.r = Res()
        def ap(s_):
            return s_._ap
    n3 = 64 * NSLOT
    MAG, Q, QF, Fs, Gc, M1 = (TV(i * n3, sh3) for i in range(6))
    QI = TV(6 * n3, sh3, I32)
    o2 = 7 * n3
    dt_, ar, thq, e1, den, bre, bim, t0, t1 = (TV(o2 + i * 64, [128, 64]) for i in range(9))
    G0 = TV(o2 + 9 * 64, [128, 64, 8])
    AC(lambda: nc.scalar.activation(out=dt_.ap(), in_=logdt.ap(), func=AF.Exp), reads=[logdt.r], writes=[dt_.r])
    V(lambda: tt(out=ar.ap(), in0=lamre.ap(), in1=dt_.ap(), op=ALU.mult), reads=[lamre.r, dt_.r], writes=[ar.r])
    V(lambda: stt(out=thq.ap(), in0=lamim.ap(), scalar=1.0 / TWO_PI, in1=dt_.ap(),
                  op0=ALU.mult, op1=ALU.mult), reads=[lamim.r, dt_.r], writes=[thq.r])
    sv_b = bc(svec.ap(), 1, sh3)
    V(lambda: tt(out=MAG.ap(), in0=bc(ar.ap(), 2, sh3), in1=sv_b, op=ALU.mult), reads=[ar.r, svec.r], writes=[MAG.r])
    AC(lambda: nc.scalar.activation(out=MAG.ap(), in_=MAG.ap(), func=AF.Exp), reads=[MAG.r], writes=[MAG.r])
    V(lambda: tt(out=Q.ap(), in0=bc(thq.ap(), 2, sh3), in1=sv_b, op=ALU.mult), reads=[thq.r, svec.r], writes=[Q.r])
    V(lambda: nc.vector.tensor_copy(out=QI.ap(), in_=Q.ap()), reads=[Q.r], writes=[QI.r])
    V(lambda: nc.vector.tensor_copy(out=QF.ap(), in_=QI.ap()), reads=[QI.r], writes=[QF.r])
    V(lambda: tt(out=Fs.ap(), in0=Q.ap(), in1=QF.ap(), op=ALU.subtract), reads=[Q.r, QF.r], writes=[Fs.r])

    def wrap(X):
        V(lambda: nc.vector.tensor_scalar(out=M1.ap(), in0=X.ap(), scalar1=0.5, scalar2=None, op0=ALU.is_gt),
          reads=[X.r], writes=[M1.r])
        V(lambda: tt(out=X.ap(), in0=X.ap(), in1=M1.ap(), op=ALU.subtract), reads=[X.r, M1.r], writes=[X.r])
        V(lambda: nc.vector.tensor_scalar(out=M1.ap(), in0=X.ap(), scalar1=-0.5, scalar2=None, op0=ALU.is_lt),
          reads=[X.r], writes=[M1.r])
        V(lambda: tt(out=X.ap(), in0=X.ap(), in1=M1.ap(), op=ALU.add), reads=[X.r, M1.r], writes=[X.r])

    wrap(Fs)
    V(lambda: nc.vector.tensor_scalar(out=Gc.ap(), in0=Fs.ap(), scalar1=0.25, scalar2=None, op0=ALU.add),
      reads=[Fs.r], writes=[Gc.r])
    wrap(Gc)
    AC(lambda: nc.scalar.activation(out=Fs.ap(), in_=Fs.ap(), func=AF.Sin, scale=TWO_PI), reads=[Fs.r], writes=[Fs.r])
    AC(lambda: nc.scalar.activation(out=Gc.ap(), in_=Gc.ap(), func=AF.Sin, scale=TWO_PI), reads=[Gc.r], writes=[Gc.r])
    V(lambda: tt(out=ERE.ap(), in0=MAG.ap(), in1=Gc.ap(), op=ALU.mult), reads=[MAG.r, Gc.r], writes=[ERE.r])
    V(lambda: tt(out=EIM.ap(), in0=MAG.ap(), in1=Fs.ap(), op=ALU.mult), reads=[MAG.r, Fs.r], writes=[EIM.r])
    f1 = EIM.ap()[:, :, 9]
    V(lambda: nc.vector.tensor_scalar(out=e1.ap(), in0=ERE.ap()[:, :, 9], scalar1=-1.0, scalar2=None, op0=ALU.add),
      reads=[ERE.r], writes=[e1.r])
    V(lambda: tt(out=den.ap(), in0=lamre.ap(), in1=lamre.ap(), op=ALU.mult), reads=[lamre.r], writes=[den.r])
    V(lambda: tt(out=t0.ap(), in0=lamim.ap(), in1=lamim.ap(), op=ALU.mult), reads=[lamim.r], writes=[t0.r])
    V(lambda: tt(out=den.ap(), in0=den.ap(), in1=t0.ap(), op=ALU.add), reads=[den.r, t0.r], writes=[den.r])
    V(lambda: nc.vector.reciprocal(out=den.ap(), in_=den.ap()), reads=[den.r], writes=[den.r])
    V(lambda: tt(out=bre.ap(), in0=e1.ap(), in1=lamre.ap(), op=ALU.mult), reads=[e1.r, lamre.r], writes=[bre.r])
    V(lambda: tt(out=t0.ap(), in0=f1, in1=lamim.ap(), op=ALU.mult), reads=[EIM.r, lamim.r], writes=[t0.r])
    V(lambda: tt(out=bre.ap(), in0=bre.ap(), in1=t0.ap(), op=ALU.add), reads=[bre.r, t0.r], writes=[bre.r])
    V(lambda: tt(out=bre.ap(), in0=bre.ap(), in1=den.ap(), op=ALU.mult), reads=[bre.r, den.r], writes=[bre.r])
    V(lambda: tt(out=bim.ap(), in0=f1, in1=lamre.ap(), op=ALU.mult), reads=[EIM.r, lamre.r], writes=[bim.r])
    V(lambda: tt(out=t1.ap(), in0=e1.ap(), in1=lamim.ap(), op=ALU.mult), reads=[e1.r, lamim.r], writes=[t1.r])
    V(lambda: tt(out=bim.ap(), in0=bim.ap(), in1=t1.ap(), op=ALU.subtract), reads=[bim.r, t1.r], writes=[bim.r])
    V(lambda: tt(out=bim.ap(), in0=bim.ap(), in1=den.ap(), op=ALU.mult), reads=[bim.r, den.r], writes=[bim.r])
    sh8 = [128, 64, 8]
    Er8, Ei8 = ERE.ap()[:, :, 0:8], EIM.ap()[:, :, 0:8]
    V(lambda: tt(out=GRE.ap(), in0=bc(bre.ap(), 2, sh8), in1=Er8, op=ALU.mult), reads=[bre.r, ERE.r], writes=[GRE.r])
    V(lambda: tt(out=G0.ap(), in0=bc(bim.ap(), 2, sh8), in1=Ei8, op=ALU.mult), reads=[bim.r, EIM.r], writes=[G0.r])
    V(lambda: tt(out=GRE.ap(), in0=GRE.ap(), in1=G0.ap(), op=ALU.subtract), reads=[GRE.r, G0.r], writes=[GRE.r])
    V(lambda: tt(out=GIM.ap(), in0=bc(bre.ap(), 2, sh8), in1=Ei8, op=ALU.mult), reads=[bre.r, EIM.r], writes=[GIM.r])
    V(lambda: tt(out=G0.ap(), in0=bc(bim.ap(), 2, sh8), in1=Er8, op=ALU.mult), reads=[bim.r, ERE.r], writes=[G0.r])
    V(lambda: tt(out=GIM.ap(), in0=GIM.ap(), in1=G0.ap(), op=ALU.add), reads=[GIM.r, G0.r], writes=[GIM.r])
    barrier(S)
    if STOP <= 2:
        return zTap

    shY = [128, NB, 8, 16]
    Yre, Yim = mk("Yre", shY, F32R), mk("Yim", shY, F32R)
    Yt = mk("Yt", shY)

    def gen_Y(pb):
        ps_ = slice(pb * NB, pb * NB + NB)
        gr = GRE.ap()[:, ps_, :].unsqueeze(3).to_broadcast(shY)
        gi = GIM.ap()[:, ps_, :].unsqueeze(3).to_broadcast(shY)
        br = Bre.ap()[:, ps_, :].unsqueeze(2).to_broadcast(shY)
        bi = Bim.ap()[:, ps_, :].unsqueeze(2).to_broadcast(shY)
        V(lambda: tt(out=Yt.ap(), in0=gi, in1=bi, op=ALU.mult), reads=[GIM.r, Bim.r], writes=[Yt.r])
        V(lambda: tt(out=Yre.ap(), in0=gr, in1=br, op=ALU.mult), reads=[GRE.r, Bre.r], writes=[Yre.r])
        V(lambda: tt(out=Yre.ap(), in0=Yre.ap(), in1=Yt.ap(), op=ALU.subtract), reads=[Yre.r, Yt.r], writes=[Yre.r])
        V(lambda: tt(out=Yt.ap(), in0=gi, in1=br, op=ALU.mult), reads=[GIM.r, Bre.r], writes=[Yt.r])
        V(lambda: tt(out=Yim.ap(), in0=gr, in1=bi, op=ALU.mult), reads=[GRE.r, Bim.r], writes=[Yim.r])
        V(lambda: tt(out=Yim.ap(), in0=Yim.ap(), in1=Yt.ap(), op=ALU.add), reads=[Yim.r, Yt.r], writes=[Yim.r])

    uc = [mk("uc%d" % i, [128, 2, 128], F32R) for i in range(2)]
    Wp = [mk("Wp%d" % i, [128, 2, 2, 128], F32R) for i in range(2)]
    V(lambda: nc.vector.memset(Yt.ap(), 0.0), writes=[Yt.r])
    for w in Wp:
        V(lambda: nc.vector.tensor_copy(out=w.ap().rearrange("p a b c -> p (a b c)"),
                                        in_=Yt.ap().rearrange("p a b c -> p (a b c)")), reads=[Yt.r], writes=[w.r])
    xch = [T.__new__(T) for _ in range(2)]
    for i in range(2):
        xch[i].r = xcb[i].r
    xch_ap = [xcb[i].ap().rearrange("p a b -> p (a b)").rearrange("p (g n) -> p g n", n=128) for i in range(2)]

    def load_xc(pb, half, b):
        S.dma('sp', 'xcb%d' % b, xch_ap[b], xc_d[:, pb * 2 * NB:(pb + 1) * 2 * NB, half * 128:(half + 1) * 128],
              writes=[xch[b].r])

    def make_uc(b, k, half, pb, ucb):
        for e in range(2):
            g = (pb * NB + k) * 2 + e
            V(lambda: stt(out=ucb.ap()[:, e, :], in0=xch_ap[b][:, 2 * k + e, :],
                          scalar=gmixc.ap()[:, g:g + 1], in1=Rt.ap()[:, half * 128:(half + 1) * 128],
                          op0=ALU.mult, op1=ALU.mult),
              reads=[xch[b].r, gmixc.r, Rt.r], writes=[ucb.r])

    def pass_B(half):
        load_xc(0, half, 0)
        for pb in range(NPB):
            b = pb % 2
            if pb + 1 < NPB:
                load_xc(pb + 1, half, 1 - b)
            gen_Y(pb)
            for k in range(NB):
                gi_ = pb * NB + k
                i2 = gi_ % 2
                ucb, wp, ps = uc[i2], Wp[i2], C.ps[i2]
                make_uc(b, k, half, pb, ucb)
                for ri, Y in enumerate((Yre, Yim)):
                    yin = Y.ap()[:, k, :, :].rearrange("p a b -> p (a b)").bitcast(F32)
                    S.op('pe', lambda: nc.tensor.transpose(ps.ap()[:, ri * 128:(ri + 1) * 128], yin, C.ident.ap()),
                         reads=[Y.r, C.ident.r], writes=[ps.r])
                for ri in range(2):
                    for e in range(2):
                        cs = slice(e * 64, e * 64 + 64)
                        AC(lambda: nc.scalar.copy(out=wp.ap()[:, ri, e, cs],
                                                  in_=ps.ap()[:, ri * 128 + e * 64: ri * 128 + e * 64 + 64]),
                           reads=[ps.r], writes=[wp.r])
                for ri in range(2):
                    for e in range(2):
                        S.op('pe', lambda: nc.tensor.matmul(ps.ap()[:, 256 + ri * 128: 256 + (ri + 1) * 128],
                                                            wp.ap()[:, ri, e, :], ucb.ap()[:, e, :],
                                                            start=(e == 0), stop=(e == 1)),
                             reads=[wp.r, ucb.r], writes=[ps.r])
                AC(lambda: nc.scalar.copy(out=Vv.ap()[:, :, gi_, :],
                                          in_=ps.ap()[:, 256:512].rearrange("p (a b) -> p a b", a=2)),
                   reads=[ps.r], writes=[Vv.r])

    Acf, Bcf = ERE.ap()[:, :, 16], EIM.ap()[:, :, 16]
    r1, r2, r3, r4 = (mk(n, [128, 64]) for n in ("r1", "r2", "r3", "r4"))
    st = [[mk("st%d%d" % (i, c), [128, 64]) for c in range(2)] for i in range(2)]
    for c in range(2):
        V(lambda: nc.vector.memset(st[0][c].ap(), 0.0), writes=[st[0][c].r])

    def pass_C(store):
        for n in range(128):
            (sr, si), (sr1, si1) = st[n % 2], st[(n + 1) % 2]
            if store:
                AC(lambda: nc.scalar.copy(out=Z.ap()[:, 0, :, n], in_=sr.ap()), reads=[sr.r], writes=[Z.r])
                AC(lambda: nc.scalar.copy(out=Z.ap()[:, 1, :, n], in_=si.ap()), reads=[si.r], writes=[Z.r])
            vr, vi = Vv.ap()[:, 0, :, n], Vv.ap()[:, 1, :, n]
            V(lambda: tt(out=r1.ap(), in0=Acf, in1=sr.ap(), op=ALU.mult), reads=[ERE.r, sr.r], writes=[r1.r])
            V(lambda: tt(out=r3.ap(), in0=Acf, in1=si.ap(), op=ALU.mult), reads=[ERE.r, si.r], writes=[r3.r])
            V(lambda: tt(out=r2.ap(), in0=Bcf, in1=si.ap(), op=ALU.mult), reads=[EIM.r, si.r], writes=[r2.r])
            V(lambda: tt(out=r4.ap(), in0=Bcf, in1=sr.ap(), op=ALU.mult), reads=[EIM.r, sr.r], writes=[r4.r])
            V(lambda: tt(out=r1.ap(), in0=r1.ap(), in1=r2.ap(), op=ALU.subtract), reads=[r1.r, r2.r], writes=[r1.r])
            V(lambda: tt(out=r3.ap(), in0=r3.ap(), in1=r4.ap(), op=ALU.add), reads=[r3.r, r4.r], writes=[r3.r])
            V(lambda: tt(out=sr1.ap(), in0=r1.ap(), in1=vr, op=ALU.add), reads=[r1.r, Vv.r], writes=[sr1.r])
            V(lambda: tt(out=si1.ap(), in0=r3.ap(), in1=vi, op=ALU.add), reads=[r3.r, Vv.r], writes=[si1.r])

    pass_B(0)
    barrier(S)
    if STOP <= 3:
        return zTap
    pass_C(False)
    pass_B(1)
    pass_C(True)
    barrier(S)
    if STOP <= 4:
        return zTap
    ZT = Vv
    ZTap = Vv.ap().rearrange("p a b c -> p (a b c)").rearrange("p (l d) -> p l d", l=8)

    Xt = mk("Xt", shX)
    WA = [mk("WA%d" % i, [128, 128], F32R) for i in range(2)]
    ytmp = [mk("ytmp%d" % i, [128, 128]) for i in range(2)]
    zg = [mk("zg%d" % i, [128, 128]) for i in range(2)]

    def gen_X(pb):
        ps_ = slice(pb * NB, pb * NB + NB)
        cr = Cre.ap()[:, ps_, :].unsqueeze(2).to_broadcast(shX)
        ci = Cim.ap()[:, ps_, :].unsqueeze(2).to_broadcast(shX)
        er = ERE.ap()[:, ps_, 8:24].unsqueeze(3).to_broadcast(shX)
        ei = EIM.ap()[:, ps_, 8:24].unsqueeze(3).to_broadcast(shX)
        V(lambda: tt(out=Xt.ap(), in0=ci, in1=ei, op=ALU.mult), reads=[Cim.r, EIM.r], writes=[Xt.r])
        V(lambda: tt(out=Xre.ap(), in0=cr, in1=er, op=ALU.mult), reads=[Cre.r, ERE.r], writes=[Xre.r])
        V(lambda: tt(out=Xre.ap(), in0=Xre.ap(), in1=Xt.ap(), op=ALU.subtract), reads=[Xt.r, Xre.r], writes=[Xre.r])
        V(lambda: tt(out=Xt.ap(), in0=ci, in1=er, op=ALU.mult), reads=[Cim.r, ERE.r], writes=[Xt.r])
        V(lambda: tt(out=Xim.ap(), in0=cr, in1=ei, op=ALU.mult), reads=[Cre.r, EIM.r], writes=[Xim.r])
        V(lambda: stt(out=Xim.ap(), in0=Xim.ap(), scalar=-1.0, in1=Xt.ap(), op0=ALU.mult, op1=ALU.subtract),
          reads=[Xt.r, Xim.r], writes=[Xim.r])

    load_xc(0, 1, 0)
    for pb in range(NPB):
        b = pb % 2
        if pb + 1 < NPB:
            load_xc(pb + 1, 1, 1 - b)
        gen_Y(pb)
        gen_X(pb)
        for k in range(NB):
            gi_ = pb * NB + k
            ucb = uc[gi_ % 2]
            make_uc(b, k, 1, pb, ucb)
            for e in range(2):
                g = gi_ * 2 + e
                i2 = g % 2
                rows = slice(e * 64, e * 64 + 64)
                ps = C.ps[2 + i2]
                psA, psY, psT = ps.ap()[:, 0:128], ps.ap()[:, 128:256], ps.ap()[:, 256:384]
                for ri, (Y, X) in enumerate(((Yre, Xre), (Yim, Xim))):
                    S.op('pe', lambda: nc.tensor.matmul(
                        psA, Y.ap()[rows, k, :, :].rearrange("p a b -> p (a b)"),
                        X.ap()[rows, k, 0:8, :].rearrange("p a b -> p (a b)"), start=(ri == 0), stop=(ri == 1)),
                         reads=[Y.r, X.r], writes=[ps.r])
                wa = WA[i2]
                V(lambda: tt(out=wa.ap(), in0=psA, in1=maskA.ap(), op=ALU.mult), reads=[ps.r, maskA.r], writes=[wa.r])
                S.op('pe', lambda: nc.tensor.matmul(psY, wa.ap(), ucb.ap()[:, e, :], start=True, stop=False),
                     reads=[wa.r, ucb.r], writes=[ps.r])
                for ri, X in enumerate((Xre, Xim)):
                    S.op('pe', lambda: nc.tensor.matmul(
                        psY, X.ap()[rows, k, 8:16, :].rearrange("p a b -> p (a b)"),
                        Z.ap()[rows, ri, gi_, :], start=False, stop=(ri == 1)),
                         reads=[X.r, Z.r], writes=[ps.r])
                yt, z_ = ytmp[i2], zg[i2]
                V(lambda: stt(out=yt.ap(), in0=ucb.ap()[:, e, :], scalar=dskipc.ap()[:, g:g + 1], in1=psY,
                              op0=ALU.mult, op1=ALU.add),
                  reads=[ucb.r, dskipc.r, ps.r], writes=[yt.r])
                AC(lambda: nc.scalar.activation(out=z_.ap(), in_=yt.ap(), func=AF.Gelu), reads=[yt.r], writes=[z_.r])
                S.op('pe', lambda: nc.tensor.transpose(psT, z_.ap(), C.ident.ap()), reads=[z_.r, C.ident.r],
                     writes=[ps.r])
                AC(lambda: nc.scalar.copy(out=ZTap[:, :, g * 16:(g + 1) * 16],
                                          in_=psT.rearrange("p (l c) -> p l c", l=8)),
                   reads=[ps.r], writes=[ZT.r])
    barrier(S)
    if STOP <= 5:
        return zTap
    zTap = Z.ap().rearrange("p a b c -> p (a b c)").rearrange("p (d t) -> p d t", d=16)
    for dc in range(16):
        for lh in range(2):
            ps = C.ps[(dc * 2 + lh) % 4]
            for li in range(4):
                l = lh * 4 + li
                S.op('pe', lambda: nc.tensor.transpose(ps.ap()[:, li * 128:(li + 1) * 128],
                                                       ZTap[:, l, dc * 128:(dc + 1) * 128], C.ident.ap()),
                     reads=[ZT.r, C.ident.r], writes=[ps.r])
            outap = zTap[:, dc, lh * 512:(lh + 1) * 512]
            if (dc + lh) % 2:
                AC(lambda: nc.scalar.copy(out=outap, in_=ps.ap()), reads=[ps.r], writes=[Z.r])
            else:
                V(lambda: nc.vector.tensor_copy(out=outap, in_=ps.ap()), reads=[ps.r], writes=[Z.r])
    print('sbuf remaining at S5 peak', nc.sbuf_bytes_remaining)
    barrier(S)
    return zTap


def s5_consts():
    svec = np.array([0, -1, -2, -3, -4, -5, -6, -7] + list(range(16)), np.float32)
    svec = np.broadcast_to(svec, (128, 24)).copy()
    j = np.arange(128) // 16
    ind = (j[:, None] == j[None, :]).astype(np.float32)
    maskA = (j[None, :] >= j[:, None]).astype(np.float32)
    ident = np.eye(128, dtype=np.float32)
    return dict(svec=svec, ind=ind, maskA=maskA, ident=ident)

def s5_params(inp):
    def gp(a):
        a = a.reshape((64, 2) + a.shape[1:])
        a = np.moveaxis(a, 0, 2)
        return np.ascontiguousarray(a.reshape((128, 64) + a.shape[3:]))
    lamre = gp(inp["lambda_re"][0]); lamim = gp(inp["lambda_im"][0])
    logdt = gp(np.broadcast_to(inp["log_dt"][0][:, None], (128, 64)))
    Bre = gp(inp["b_re"][0]); Bim = gp(inp["b_im"][0])
    Cre = gp(np.transpose(inp["c_re"][0], (0, 2, 1))); Cim = gp(np.transpose(inp["c_im"][0], (0, 2, 1)))
    gm = inp["g_mix"][0].reshape(128, 16)
    gmixc = np.ascontiguousarray(np.broadcast_to(gm.T[None, :, :], (8, 16, 128)).reshape(128, 128))
    ds = inp["d_skip"][0].reshape(128, 16)
    dskipc = np.ascontiguousarray(np.broadcast_to(ds.T[None, :, :], (8, 16, 128)).reshape(128, 128))
    return dict(lamre=lamre, lamim=lamim, logdt=logdt, Bre=Bre, Bim=Bim, Cre=Cre, Cim=Cim, gmixc=gmixc, dskipc=dskipc)

def xc_layout(x, b, half):
    if half == 1:
        win = x[b]
    else:
        win = np.concatenate([np.zeros((1024, 2048), np.float32), x[b, :1024]], 0)
    w = win.reshape(256, 8, 128, 16)
    return np.ascontiguousarray(np.transpose(w, (1, 3, 2, 0)).reshape(128, 128, 256))

def fm(a):
    t = a.shape[0]
    return np.ascontiguousarray(np.transpose(a.reshape(t, 16, 128), (2, 1, 0)))

def unfm(a):
    return np.ascontiguousarray(np.transpose(a, (2, 1, 0)).reshape(a.shape[2], 2048))


AX = mybir.AxisListType
BIG = 30000.0
SCALE = 128.0 ** -0.5


def proj_fm(S, nc, C, w_d, nchunks, src_ap, src_res, sink):
    S.dma('pool', 'wgu0', C.wgu[0].ap()[:, 0, :, :], w_d[0], writes=[C.wgu[0].r])
    for oc in range(nchunks):
        sl = oc % 2
        if oc + 1 < nchunks:
            S.dma('pool', 'wgu%d' % (1 - sl), C.wgu[1 - sl].ap()[:, 0, :, :], w_d[oc + 1], writes=[C.wgu[1 - sl].r])
        w = C.wgu[sl]
        for th in range(2):
            tsl = slice(th * 512, (th + 1) * 512)
            ps = C.ps[(oc * 2 + th) % 4]
            for dc in range(16):
                S.op('pe', lambda: nc.tensor.matmul(ps.ap(), w.ap()[:, 0, dc, :], src_ap[:, dc, tsl],
                                                    start=(dc == 0), stop=(dc == 15)),
                     reads=[w.r, src_res], writes=[ps.r], inc=(dc == 15))
            sink(oc, th, tsl, ps)


def attention(S, nc, C, D_, AT, es):
    def mk(name, shape, dt=F32):
        t = T.__new__(T)
        t.t = es.enter_context(nc.sbuf_tensor("sb_" + name, list(shape), dt))
        t.r = Res(name)
        t.shape = shape
        return t
    V = lambda *a, **k: S.op('dve', *a, **k)
    AC = lambda *a, **k: S.op('act', *a, **k)
    tt = nc.vector.tensor_tensor
    stt = nc.vector.scalar_tensor_tensor
    KT = [mk("KT%d" % i, [128, 2048], F32R) for i in range(2)]
    VH = [mk("VH%d" % i, [128, 16, 128], F32R) for i in range(2)]
    QH = [mk("QH%d" % i, [128, 1024], F32R) for i in range(2)]
    Dw, Cw = mk("Dw", [128, 2560]), mk("Cw", [128, 1024])
    E = mk("E", [8, 8, 128], F32R)
    gmask, keep, ownb = mk("gmask", [128, 8, 8]), mk("keep", [128, 8, 8]), mk("ownb", [128, 8, 8])
    PT = [mk("PT%d" % i, [128, 512], F32R) for i in range(2)]
    tmp = [mk("tmp%d" % i, [128, 512]) for i in range(2)]
    SBT = [mk("SBT%d" % i, [8, 1024], F32R) for i in range(2)]
    kms = mk("kms", [128, 8])
    kmT = [mk("kmT%d" % i, [128, 8], F32R) for i in range(2)]
    Gm, mx, sel, selb = (mk(n, [128, 8, 8]) for n in ("Gm", "mx", "sel", "selb"))
    rden = mk("rden", [128, 512])
    for t, nm in ((Dw, "Dw"), (Cw, "Cw"), (gmask, "gmask"), (keep, "keep"), (ownb, "ownb")):
        S.dma('sp', 'ld_' + nm, t.ap(), D_[nm], writes=[t.r])
    S.dma('pool', 'ld_E', E.ap(), D_["E"], writes=[E.r])

    def load_head(h):
        sl = h % 2
        S.dma('pool', 'KT%d' % sl, KT[sl].ap(), D_["KT"][h], writes=[KT[sl].r])
        S.dma('pool', 'VH%d' % sl, VH[sl].ap(), D_["V"][h], writes=[VH[sl].r])
        S.dma('pool', 'QH%d' % sl, QH[sl].ap(), D_["qT"][h], writes=[QH[sl].r])

    load_head(0)
    for h in range(16):
        sl = h % 2
        kt_, vh, qh, sbt, km = KT[sl], VH[sl], QH[sl], SBT[sl], kmT[sl]
        if h + 1 < 16:
            load_head(h + 1)
        slope = 2.0 ** (-(h + 1) / 2.0)
        V(lambda: nc.vector.tensor_reduce(out=kms.ap(), in_=kt_.ap().bitcast(F32).rearrange("p (s k) -> p s k", k=256),
                                          axis=AX.X, op=ALU.add), reads=[kt_.r], writes=[kms.r])
        V(lambda: nc.vector.tensor_scalar(out=km.ap(), in0=kms.ap(), scalar1=1.0 / 256, scalar2=None, op0=ALU.mult),
          reads=[kms.r], writes=[km.r])
        psG, psT = C.ps[6], C.ps[7]
        for qt in range(8):
            S.op('pe', lambda: nc.tensor.matmul(psG.ap()[:, qt * 8:(qt + 1) * 8], qh.ap()[:, qt * 128:(qt + 1) * 128],
                                                km.ap(), start=True, stop=True),
                 reads=[qh.r, km.r], writes=[psG.r])
        V(lambda: tt(out=Gm.ap(), in0=psG.ap()[:, 0:64].rearrange("p (a b) -> p a b", a=8), in1=gmask.ap(), op=ALU.add),
          reads=[psG.r, gmask.r], writes=[Gm.r])
        for qt in range(8):
            V(lambda: nc.vector.max(out=mx.ap()[:, qt, :], in_=Gm.ap()[:, qt, :]), reads=[Gm.r], writes=[mx.r])
        V(lambda: tt(out=sel.ap(), in0=Gm.ap(), in1=mx.ap()[:, :, 2:3].to_broadcast([128, 8, 8]), op=ALU.is_ge),
          reads=[Gm.r, mx.r], writes=[sel.r])
        V(lambda: tt(out=sel.ap(), in0=sel.ap(), in1=keep.ap(), op=ALU.mult), reads=[sel.r, keep.r], writes=[sel.r])
        V(lambda: tt(out=sel.ap(), in0=sel.ap(), in1=keep.ap(), op=ALU.subtract), reads=[sel.r, keep.r], writes=[sel.r])
        V(lambda: stt(out=selb.ap(), in0=sel.ap(), scalar=BIG, in1=ownb.ap(), op0=ALU.mult, op1=ALU.add),
          reads=[sel.r, ownb.r], writes=[selb.r])
        for qt in range(8):
            pst = psT if qt < 4 else psG
            S.op('pe', lambda: nc.tensor.transpose(pst.ap()[0:8, (qt % 4) * 128:(qt % 4 + 1) * 128], selb.ap()[:, qt, :],
                                                   C.ident.ap()),
                 reads=[selb.r, C.ident.r], writes=[pst.r])
        AC(lambda: nc.scalar.copy(out=sbt.ap()[:, 0:512], in_=psT.ap()[0:8, :]), reads=[psT.r], writes=[sbt.r])
        AC(lambda: nc.scalar.copy(out=sbt.ap()[:, 512:1024], in_=psG.ap()[0:8, :]), reads=[psG.r], writes=[sbt.r])
        for Q in range(2):
            qsl = slice(Q * 512, (Q + 1) * 512)
            par = (h * 2 + Q) % 2
            psO, psD = C.ps[2 + 2 * par], C.ps[3 + 2 * par]
            kts = list(range(8)) + [8 + i for i in range(4 * Q + 4)]

            def emit_S(idx):
                kt = kts[idx]
                ps = C.ps[idx % 2]
                S.op('pe', lambda: nc.tensor.matmul(ps.ap(), kt_.ap()[:, kt * 128:(kt + 1) * 128], qh.ap()[:, qsl],
                                                    start=True, stop=False),
                     reads=[kt_.r, qh.r], writes=[ps.r], inc=False)
                S.op('pe', lambda: nc.tensor.matmul(ps.ap(), E.ap()[:, kt // 2, :], sbt.ap()[:, qsl],
                                                    start=False, stop=True),
                     reads=[E.r, sbt.r], writes=[ps.r])

            emit_S(0)
            for idx, kt in enumerate(kts):
                if idx + 1 < len(kts):
                    emit_S(idx + 1)
                ps = C.ps[idx % 2]
                i2 = idx % 2
                off = 1024 + Q * 512 - kt * 128 + 512
                V(lambda: stt(out=tmp[i2].ap(), in0=Dw.ap()[:, off:off + 512], scalar=slope / SCALE, in1=ps.ap(),
                              op0=ALU.mult, op1=ALU.add),
                  reads=[Dw.r, ps.r], writes=[tmp[i2].r])
                if kt >= 8 and (kt - 8) >= 4 * Q:
                    V(lambda: tt(out=tmp[i2].ap(), in0=tmp[i2].ap(), in1=Cw.ap()[:, off:off + 512], op=ALU.add),
                      reads=[tmp[i2].r, Cw.r], writes=[tmp[i2].r])
                AC(lambda: nc.scalar.activation(out=PT[i2].ap(), in_=tmp[i2].ap(), func=AF.Exp, scale=SCALE),
                   reads=[tmp[i2].r], writes=[PT[i2].r])
                first, last = idx == 0, idx == len(kts) - 1
                S.op('pe', lambda: nc.tensor.matmul(psO.ap(), vh.ap()[:, kt, :], PT[i2].ap(), start=first, stop=last),
                     reads=[vh.r, PT[i2].r], writes=[psO.r], inc=False)
                S.op('pe', lambda: nc.tensor.matmul(psD.ap(), C.ones.ap(), PT[i2].ap(), start=first, stop=last),
                     reads=[C.ones.r, PT[i2].r], writes=[psD.r])
            V(lambda: nc.vector.reciprocal(out=rden.ap(), in_=psD.ap()), reads=[psD.r], writes=[rden.r])
            V(lambda: tt(out=AT.ap()[:, h, qsl], in0=psO.ap(), in1=rden.ap(), op=ALU.mult),
              reads=[psO.r, rden.r], writes=[AT.r])
    barrier(S)


def alloc_common(S, nc, C, D_, mk):
    C.ps = [T(nc, "ps%d" % i, [128, 512], F32, psum=True) for i in range(8)]
    C.ident = mk("ident_sb", [128, 128]); C.epsc = mk("epsc", [128, 1])
    S.dma('sp', 'ld_ident', C.ident.ap(), D_["ident"], writes=[C.ident.r])
    S.op('dve', lambda: nc.vector.memset(C.epsc.ap(), EPS), writes=[C.epsc.r])
    onesf = mk("onesf", [128, 128]); C.ones = mk("ones", [128, 128], F32R)
    S.op('dve', lambda: nc.vector.memset(onesf.ap(), 1.0), writes=[onesf.r])
    S.op('dve', lambda: nc.vector.tensor_copy(out=C.ones.ap(), in_=onesf.ap()), reads=[onesf.r], writes=[C.ones.r])
    C.gc = mk("gc", [128, 16])


def alloc_ffn(nc, C, mk):
    C.sq = [mk("sq%d" % i, [128, 1024], F32R) for i in range(2)]
    C.rbc = mk("rbc", [128, 1024])
    C.wgu = [mk("wgu%d" % i, [128, 2, 16, 128], F32R) for i in range(2)]
    C.wd = [mk("wd%d" % i, [128, 2048], F32R) for i in range(2)]
    C.act = [mk("act%d" % i, [128, 1024], F32R) for i in range(2)]
    C.sil = [mk("sil%d" % i, [128, 512]) for i in range(2)]


def build_l2():
    nc = bass.Bass("TRN2", target_bir_lowering=False)
    S = Sched(nc)

    class C:
        pass
    dshapes = dict(hT=[128, 16, 1024], qT=[16, 128, 1024], KT=[16, 128, 2048], V=[16, 128, 16, 128],
                   Dw=[128, 2560], Cw=[128, 1024], gmask=[128, 8, 8], keep=[128, 8, 8], ownb=[128, 8, 8],
                   E=[8, 8, 128], ident=[128, 128], wo=[16, 128, 16, 128], gffn=[128, 16], gfin=[128, 16],
                   wgu=[44, 128, 2, 16, 128], wd=[44, 128, 2048])
    D_ = {k: nc.dram_tensor(k, v, F32, kind="ExternalInput").ap() for k, v in dshapes.items()}
    out = nc.dram_tensor("oT", [128, 16, 1024], F32, kind="ExternalOutput").ap()
    mk = lambda name, shape, dt=F32: T(nc, name, shape, dt)
    alloc_common(S, nc, C, D_, mk)
    AT = mk("AT", [128, 16, 1024], F32R)
    with ExitStack() as es:
        attention(S, nc, C, D_, AT, es)
        print("sbuf remaining (attention)", nc.sbuf_bytes_remaining)
    h = mk("h", [128, 16, 1024])
    S.dma('sp', 'ld_h', h.ap(), D_["hT"], writes=[h.r])
    alloc_ffn(nc, C, mk)
    print("sbuf remaining (ffn)", nc.sbuf_bytes_remaining)

    def sink_o(oc, th, tsl, ps):
        S.op('dve', lambda: nc.vector.tensor_tensor(out=h.ap()[:, oc, tsl], in0=h.ap()[:, oc, tsl], in1=ps.ap(),
                                                    op=ALU.add), reads=[ps.r, h.r], writes=[h.r])
    proj_fm(S, nc, C, D_["wo"], 16, AT.ap(), AT.r, sink_o)
    barrier(S)
    S.dma('sp', 'ld_gc', C.gc.ap(), D_["gffn"], writes=[C.gc.r])
    rmsnorm_fm(S, nc, C, h, C.gc.ap(), AT)
    barrier(S)
    ffn(S, nc, C, h, AT, D_["wgu"], D_["wd"])
    barrier(S)
    S.dma('sp', 'ld_gc', C.gc.ap(), D_["gfin"], writes=[C.gc.r])
    rmsnorm_fm(S, nc, C, h, C.gc.ap(), h)
    ev = S.dma('sp', 'st_out', out, h.ap(), reads=[h.r])
    S._wait('sp', ev)
    print({k: v for k, v in S.cnt.items()})
    return nc


def l2_consts(half):
    i = np.arange(128, dtype=np.float32)[:, None]
    m = np.arange(2560, dtype=np.float32)[None, :]
    Dw = (i - m + 512).astype(np.float32)
    Cw = np.where(Dw[:, :1024] > 0, -BIG, 0.0).astype(np.float32)
    gmask = np.full((8, 8), -1e30, np.float32); keep = np.zeros((8, 8), np.float32); ownb = np.full((8, 8), -BIG, np.float32)
    for qt in range(8):
        o = qt // 2
        valid = ([0, 1, 2, 3] if half == 1 else []) + [4 + k for k in range(o)]
        for s in valid:
            gmask[qt, s] = 0.0; keep[qt, s] = 1.0; ownb[qt, s] = 0.0
        ownb[qt, 4 + o] = 0.0
    rep = lambda a: np.ascontiguousarray(np.broadcast_to(a[None], (128, 8, 8)))
    E = np.zeros((8, 8, 128), np.float32)
    for s in range(8):
        E[s, s, :] = 1.0
    return dict(Dw=Dw, Cw=Cw, gmask=rep(gmask), keep=rep(keep), ownb=rep(ownb), E=E, ident=np.eye(128, dtype=np.float32))


def ffn_weights(inp, i):
    wg = inp["w_gate"][i].reshape(16, 128, 44, 128).transpose(2, 1, 0, 3)
    wu = inp["w_up"][i].reshape(16, 128, 44, 128).transpose(2, 1, 0, 3)
    wgu = np.ascontiguousarray(np.stack([wg, wu], 2))
    wd = np.ascontiguousarray(inp["w_down"][i].reshape(44, 128, 2048))
    return wgu, wd

def chunked(w):
    F = w.shape[1]
    return np.ascontiguousarray(w.reshape(16, 128, F // 128, 128).transpose(2, 1, 0, 3))

colz = lambda v: np.ascontiguousarray(v.reshape(16, 128).T)

def l2_maps(inp, h2, q, k, v, cores):
    wgu, wd = ffn_weights(inp, 1)
    wo = chunked(inp["w_o"][0])
    maps = []
    for c in cores:
        b, half = c // 2, c % 2
        m = l2_consts(half)
        own = slice(half * 1024, (half + 1) * 1024)
        prev = slice(0, 1024)
        m["hT"] = fm(h2[b, own])
        m["qT"] = np.ascontiguousarray(q[b, :, own, :].transpose(0, 2, 1))
        kw = np.concatenate([k[b, :, prev], k[b, :, own]], 1)
        vw = np.concatenate([v[b, :, prev], v[b, :, own]], 1)
        m["KT"] = np.ascontiguousarray(kw.transpose(0, 2, 1))
        m["V"] = np.ascontiguousarray(vw.reshape(16, 16, 128, 128).transpose(0, 2, 1, 3))
        m.update(wo=wo, gffn=colz(inp["g_ffn"][1]), gfin=colz(inp["g_final"]), wgu=wgu, wd=wd)
        maps.append(m)
    return maps


def build_l1(with_proj=True):
    nc = bass.Bass("TRN2", target_bir_lowering=False)
    S = Sched(nc)

    class C:
        pass
    dshapes = dict(lamre=[128, 64], lamim=[128, 64], logdt=[128, 64], Bre=[128, 64, 16], Bim=[128, 64, 16],
                   Cre=[128, 64, 16], Cim=[128, 64, 16], gmixc=[128, 128], dskipc=[128, 128], svec=[128, 24],
                   maskA=[128, 128], ind=[128, 128], xc=[128, 128, 256], ident=[128, 128], xT=[128, 16, 1024],
                   wglu=[16, 128, 16, 128], bglu=[128, 16], gffn=[128, 16], wgu=[44, 128, 2, 16, 128],
                   wd=[44, 128, 2048], gkv=[128, 16], gmix1=[128, 16], wkv=[32, 128, 16, 128], wq=[16, 128, 16, 128])
    D_ = {k: nc.dram_tensor(k, v, F32, kind="ExternalInput").ap() for k, v in dshapes.items()}
    out = nc.dram_tensor("hT", [128, 16, 1024], F32, kind="ExternalOutput").ap()
    kv_out = nc.dram_tensor("kvT", [32, 128, 1024], F32, kind="ExternalOutput").ap()
    q_out = nc.dram_tensor("qTo", [16, 128, 1024], F32, kind="ExternalOutput").ap()
    mk = lambda name, shape, dt=F32: T(nc, name, shape, dt)
    alloc_common(S, nc, C, D_, mk)
    Z = mk("Zst", [128, 2, 64, 128], F32R)
    with ExitStack() as es:
        zTap = s5_mixer(S, nc, C, D_, Z, es)
    h = mk("h", [128, 16, 1024])
    S.dma('sp', 'ld_h', h.ap(), D_["xT"], writes=[h.r])
    bglu = mk("bglu_sb", [128, 16])
    S.dma('sp', 'ld_bglu', bglu.ap(), D_["bglu"], writes=[bglu.r])
    alloc_ffn(nc, C, mk)
    print("sbuf remaining", nc.sbuf_bytes_remaining)

    class U:
        pass
    uu = U(); uu.r = Z.r; uu.ap = lambda: zTap

    def sink_glu(oc, th, tsl, ps):
        sil = C.sil[th]
        S.op('act', lambda: nc.scalar.activation(out=sil.ap(), in_=ps.ap(), func=AF.Sigmoid,
                                                 bias=bglu.ap()[:, oc:oc + 1], scale=1.0),
             reads=[ps.r, bglu.r], writes=[sil.r])
        S.op('dve', lambda: nc.vector.tensor_tensor(out=sil.ap(), in0=sil.ap(), in1=zTap[:, oc, tsl], op=ALU.mult),
             reads=[sil.r, Z.r], writes=[sil.r])
        S.op('dve', lambda: nc.vector.tensor_tensor(out=h.ap()[:, oc, tsl], in0=h.ap()[:, oc, tsl], in1=sil.ap(),
                                                    op=ALU.add), reads=[sil.r, h.r], writes=[h.r])
    proj_fm(S, nc, C, D_["wglu"], 16, zTap, Z.r, sink_glu)
    barrier(S)
    S.dma('sp', 'ld_gc', C.gc.ap(), D_["gffn"], writes=[C.gc.r])
    rmsnorm_fm(S, nc, C, h, C.gc.ap(), uu)
    barrier(S)
    ffn(S, nc, C, h, uu, D_["wgu"], D_["wd"])
    barrier(S)
    evs = [S.dma('sp', 'st_out', out, h.ap(), reads=[h.r])]
    if with_proj:
        def make_sink(dst, slotname):
            def sink(oc, th, tsl, ps):
                st = C.act[oc % 2]
                S.op('act', lambda: nc.scalar.copy(out=st.ap()[:, tsl], in_=ps.ap()), reads=[ps.r], writes=[st.r])
                if th == 1:
                    evs.append(S.dma('sp', slotname + str(oc % 2), dst[oc], st.ap().bitcast(F32), reads=[st.r]))
            return sink
        S.dma('sp', 'ld_gc', C.gc.ap(), D_["gkv"], writes=[C.gc.r])
        rmsnorm_fm(S, nc, C, h, C.gc.ap(), uu)
        proj_fm(S, nc, C, D_["wkv"], 32, zTap, Z.r, make_sink(kv_out, "st_kv"))
        barrier(S)
        S.dma('sp', 'ld_gc', C.gc.ap(), D_["gmix1"], writes=[C.gc.r])
        rmsnorm_fm(S, nc, C, h, C.gc.ap(), uu)
        proj_fm(S, nc, C, D_["wq"], 16, zTap, Z.r, make_sink(q_out, "st_q"))
    barrier(S)
    print({k: v for k, v in S.cnt.items()})
    return nc


PERM = (np.arange(128)[None, :] * 8 + np.arange(8)[:, None]).reshape(-1)
INV = np.argsort(PERM)


def l1_maps(inp, cores):
    cons = s5_consts(); par = s5_params(inp)
    wglu = chunked(inp["w_glu"][0]); wkv = chunked(inp["w_kv"]); wq = chunked(inp["w_q"][0])
    wgu, wd = ffn_weights(inp, 0)
    maps = []
    for c in cores:
        b, half = c // 2, c % 2
        m = dict(cons); m.update(par); m["xc"] = xc_layout(inp["x"], b, half)
        m["xT"] = fm(inp["x"][b, half * 1024:(half + 1) * 1024][PERM])
        m.update(wglu=wglu, bglu=colz(inp["b_glu"][0]), gffn=colz(inp["g_ffn"][0]), wgu=wgu, wd=wd,
                 gkv=colz(inp["g_kv"]), gmix1=colz(inp["g_mix"][1]), wkv=wkv, wq=wq)
        maps.append(m)
    return maps


def kernel(**inp):
    inp = {k: np.ascontiguousarray(np.asarray(v), dtype=np.float32) for k, v in inp.items()}
    cores = list(range(8))
    nc1 = build_l1()
    r1 = run_bass_kernel_spmd(nc1, l1_maps(inp, cores), core_ids=cores)
    h2 = np.zeros((4, 2048, 2048), np.float32)
    q = np.zeros((4, 16, 2048, 128), np.float32); k = np.zeros_like(q); v = np.zeros_like(q)
    for c in cores:
        b, half = c // 2, c % 2
        own = slice(half * 1024, (half + 1) * 1024)
        res = r1.results[c]
        h2[b, own] = unfm(res["hT"])[INV]
        kv = res["kvT"][:, :, INV]
        k[b, :, own, :] = kv[:16].transpose(0, 2, 1)
        v[b, :, own, :] = kv[16:].transpose(0, 2, 1)
        q[b, :, own, :] = res["qTo"][:, :, INV].transpose(0, 2, 1)
    nc2 = build_l2()
    r2 = run_bass_kernel_spmd(nc2, l2_maps(inp, h2, q, k, v, cores), core_ids=cores)
    out = np.zeros((4, 2048, 2048), np.float32)
    for c in cores:
        b, half = c // 2, c % 2
        out[b, half * 1024:(half + 1) * 1024] = unfm(r2.results[c]["oT"])
    return out
```
